# Optimizing a Trainium2 kernel written in Bass

```python
import math
import jax
import jax.numpy as jnp
from jax import lax
import numpy as np


D_MODEL = 1024
BATCH = 8
SEQ = 4096
DEPTH = 1

MEM_LEN = 256
CONV_CH = D_MODEL
CONV_K = 3
POOL_WINDOWS = (2, 4, 8, 16)
N_POOL_GROUPS = 4
POOL_GROUP_DIM = D_MODEL // N_POOL_GROUPS
POOL_WIDTH = N_POOL_GROUPS * POOL_GROUP_DIM
X_HEADS = 4
X_HEAD_DIM = D_MODEL // X_HEADS
X_WIDTH = X_HEADS * X_HEAD_DIM
N_BRANCH = 3
IN_SPLITS = (CONV_CH, 2 * CONV_CH, 3 * CONV_CH, 3 * CONV_CH + POOL_WIDTH, 3 * CONV_CH + POOL_WIDTH + X_WIDTH)
IN_COLS = 3 * CONV_CH + POOL_WIDTH + X_WIDTH + N_BRANCH * D_MODEL
N_GROUPS = 8
EXPERTS_PER_GROUP = 8
N_EXPERTS = N_GROUPS * EXPERTS_PER_GROUP
TOP_K = 2
D_EXPERT = D_MODEL // 2
MOE_BLOCK = 128
ALPHA = (2.0 * DEPTH) ** 0.25
BETA = (8.0 * DEPTH) ** -0.25
LN_EPS = 1e-5

kernel_name = 'hybrid_conv_pool_memory_hmoe'


def layer_norm(x, g, b):
    xf = x.astype(jnp.float32)
    mu = jnp.mean(xf, axis=-1, keepdims=True)
    var = jnp.mean(jnp.square(xf - mu), axis=-1, keepdims=True)
    y = (xf - mu) * lax.rsqrt(var + LN_EPS)
    return (y * g.astype(jnp.float32) + b.astype(jnp.float32)).astype(x.dtype)


def short_conv_branch(b_gate, c_gate, h, conv_w, w_conv_out):
    u = c_gate * h
    v = lax.conv_general_dilated(
        u, conv_w[:, None, :].astype(u.dtype), window_strides=(1,), padding=[(CONV_K - 1, 0)],
        dimension_numbers=('NWC', 'WIO', 'NWC'), feature_group_count=CONV_CH)
    return jnp.einsum('bsc,cd->bsd', b_gate * v, w_conv_out)


def multiscale_pool_branch(p, w_pool, pool_scale):
    bn, s, _ = p.shape
    pf = p.reshape(bn, s, N_POOL_GROUPS, POOL_GROUP_DIM).astype(jnp.float32)
    cs = jnp.cumsum(pf, axis=1)
    cs = jnp.concatenate([jnp.zeros_like(cs[:, :1]), cs], axis=1)
    pos = jnp.arange(1, s + 1, dtype=jnp.float32)
    means = []
    for gi, w in enumerate(POOL_WINDOWS):
        upper = cs[:, 1:, gi]
        lower = jnp.concatenate([jnp.zeros_like(cs[:, :w - 1, gi]), cs[:, :s - w + 1, gi]], axis=1)
        count = jnp.minimum(pos, float(w))[None, :, None]
        means.append((upper - lower) / count)
    d = (jnp.stack(means, axis=2) - pf).astype(p.dtype)
    y = jnp.einsum('bsgc,gcd->bsgd', d, w_pool).reshape(bn, s, POOL_WIDTH)
    return y * pool_scale


def memory_cross_attention(q, mem, w_kv, w_xo):
    bn, s, _ = q.shape
    m = mem.shape[1]
    kv = jnp.einsum('bmd,de->bme', mem, w_kv)
    k, v = jnp.split(kv, 2, axis=-1)
    qh = q.reshape(bn, s, X_HEADS, X_HEAD_DIM)
    kh = k.reshape(bn, m, X_HEADS, X_HEAD_DIM)
    vh = v.reshape(bn, m, X_HEADS, X_HEAD_DIM)
    sc = jnp.einsum('bshd,bmhd->bhsm', qh, kh).astype(jnp.float32) * (X_HEAD_DIM ** -0.5)
    a = jax.nn.softmax(sc, axis=-1).astype(q.dtype)
    o = jnp.einsum('bhsm,bmhd->bshd', a, vh).reshape(bn, s, X_WIDTH)
    return jnp.einsum('bse,ed->bsd', o, w_xo)


def hybrid_mixer(x, mem, w_in, b_gate, conv_w, w_conv_out, w_pool, pool_scale, w_kv, w_xo, w_o):
    bn, s, _ = x.shape
    z = jnp.einsum('bsd,de->bse', x, w_in)
    cb, cc, ch, pz, q, gz = jnp.split(z, IN_SPLITS, axis=-1)
    gates = jax.nn.sigmoid((gz + b_gate).astype(jnp.float32)).astype(x.dtype).reshape(bn, s, N_BRANCH, D_MODEL)
    y_conv = short_conv_branch(cb, cc, ch, conv_w, w_conv_out)
    y_pool = multiscale_pool_branch(pz, w_pool, pool_scale)
    y_mem = memory_cross_attention(q, mem, w_kv, w_xo)
    merged = gates[:, :, 0] * y_conv + gates[:, :, 1] * y_pool + gates[:, :, 2] * y_mem
    return jnp.einsum('bsd,de->bse', merged, w_o)


def hierarchical_moe(x, w_rg, b_rg, w_re, b_re, w_up, w_down):
    bn, s, d = x.shape
    n = bn * s
    xt = x.reshape(n, d)
    g_prob = jax.nn.softmax(jnp.einsum('nd,dg->ng', xt, w_rg).astype(jnp.float32) + b_rg.astype(jnp.float32), axis=-1)
    g_sel = jnp.argmax(g_prob, axis=-1)
    g_w = jnp.take_along_axis(g_prob, g_sel[:, None], axis=-1)
    e_all = (jnp.einsum('nd,de->ne', xt, w_re).astype(jnp.float32) + b_re.astype(jnp.float32)).reshape(n, N_GROUPS, EXPERTS_PER_GROUP)
    e_logits = jnp.take_along_axis(e_all, g_sel[:, None, None], axis=1)[:, 0]
    top_l, top_i = lax.top_k(e_logits, TOP_K)
    top_w = jax.nn.softmax(top_l, axis=-1) * g_w
    expert_id = (g_sel[:, None] * EXPERTS_PER_GROUP + top_i).reshape(-1)
    a_n = n * TOP_K
    n_blocks = (a_n + N_EXPERTS * (MOE_BLOCK - 1) + MOE_BLOCK - 1) // MOE_BLOCK
    p_rows = n_blocks * MOE_BLOCK
    order = jnp.argsort(expert_id)
    sorted_e = expert_id[order]
    counts = jnp.bincount(expert_id, length=N_EXPERTS)
    starts = jnp.cumsum(counts) - counts
    padded = (counts + MOE_BLOCK - 1) // MOE_BLOCK * MOE_BLOCK
    pends = jnp.cumsum(padded)
    pstarts = pends - padded
    dest_sorted = pstarts[sorted_e] + (jnp.arange(a_n) - starts[sorted_e])
    dest = jnp.zeros((a_n,), dtype=dest_sorted.dtype).at[order].set(dest_sorted)
    tok = jnp.arange(a_n) // TOP_K
    x_buf = jnp.zeros((p_rows, d), dtype=x.dtype).at[dest].set(xt[tok])
    block_e = jnp.minimum(jnp.searchsorted(pends, jnp.arange(n_blocks) * MOE_BLOCK, side='right'), N_EXPERTS - 1)

    def expert_block(args):
        xb, e = args
        hg, hv = jnp.split(xb @ w_up[e], 2, axis=-1)
        return (jax.nn.silu(hg) * hv) @ w_down[e]

    y_buf = lax.map(expert_block, (x_buf.reshape(n_blocks, MOE_BLOCK, d), block_e))
    y_assign = y_buf.reshape(p_rows, d)[dest].reshape(n, TOP_K, d)
    y = jnp.einsum('nkd,nk->nd', y_assign, top_w.astype(x.dtype))
    return y.reshape(bn, s, d)


def setup_inputs(seed: int = 0) -> dict:
    key = jax.random.key(seed)
    ks = jax.random.split(key, 24)
    L = DEPTH
    nrm = lambda k, shape: jax.random.normal(k, shape, dtype=jnp.float32)
    return {
        'x': nrm(ks[0], (BATCH, SEQ, D_MODEL)),
        'mem': nrm(ks[1], (BATCH, MEM_LEN, D_MODEL)),
        'w_in': nrm(ks[2], (L, D_MODEL, IN_COLS)) * D_MODEL ** -0.5,
        'b_gate': 0.1 * nrm(ks[3], (L, N_BRANCH * D_MODEL)),
        'conv_w': nrm(ks[4], (L, CONV_K, CONV_CH)) * CONV_K ** -0.5,
        'w_conv_out': nrm(ks[5], (L, CONV_CH, D_MODEL)) * CONV_CH ** -0.5,
        'w_pool': nrm(ks[6], (L, N_POOL_GROUPS, POOL_GROUP_DIM, POOL_GROUP_DIM)) * POOL_GROUP_DIM ** -0.5,
        'pool_scale': 1.0 + 0.1 * nrm(ks[7], (L, POOL_WIDTH)),
        'w_kv': nrm(ks[8], (L, D_MODEL, 2 * X_WIDTH)) * D_MODEL ** -0.5,
        'w_xo': nrm(ks[9], (L, X_WIDTH, D_MODEL)) * X_WIDTH ** -0.5,
        'w_o': nrm(ks[10], (L, D_MODEL, D_MODEL)) * (D_MODEL ** -0.5 * BETA),
        'ln1_g': 1.0 + 0.02 * nrm(ks[11], (L, D_MODEL)),
        'ln1_b': 0.02 * nrm(ks[12], (L, D_MODEL)),
        'w_router_group': nrm(ks[13], (L, D_MODEL, N_GROUPS)) * D_MODEL ** -0.5,
        'b_router_group': 0.01 * nrm(ks[14], (L, N_GROUPS)),
        'w_router_expert': nrm(ks[15], (L, D_MODEL, N_EXPERTS)) * D_MODEL ** -0.5,
        'b_router_expert': 0.01 * nrm(ks[16], (L, N_EXPERTS)),
        'w_up': nrm(ks[17], (L, N_EXPERTS, D_MODEL, 2 * D_EXPERT)) * D_MODEL ** -0.5,
        'w_down': nrm(ks[18], (L, N_EXPERTS, D_EXPERT, D_MODEL)) * (D_EXPERT ** -0.5 * BETA),
        'ln2_g': 1.0 + 0.02 * nrm(ks[19], (L, D_MODEL)),
        'ln2_b': 0.02 * nrm(ks[20], (L, D_MODEL)),
    }


def reference(x, mem, w_in, b_gate, conv_w, w_conv_out, w_pool, pool_scale, w_kv, w_xo, w_o, ln1_g, ln1_b,
              w_router_group, b_router_group, w_router_expert, b_router_expert, w_up, w_down, ln2_g, ln2_b):
    h = x
    for l in range(DEPTH):
        mix = hybrid_mixer(h, mem, w_in[l], b_gate[l], conv_w[l], w_conv_out[l], w_pool[l], pool_scale[l],
                           w_kv[l], w_xo[l], w_o[l])
        h = layer_norm(ALPHA * h + mix, ln1_g[l], ln1_b[l])
        ffn = hierarchical_moe(h, w_router_group[l], b_router_group[l], w_router_expert[l], b_router_expert[l],
                               w_up[l], w_down[l])
        h = layer_norm(ALPHA * h + ffn, ln2_g[l], ln2_b[l])
    return h
```

```python
from contextlib import ExitStack

import numpy as np
import concourse.bass as bass
import concourse.mybir as mybir
from concourse.bass_utils import run_bass_kernel_spmd

F32 = mybir.dt.float32
BF16 = mybir.dt.bfloat16
I32 = mybir.dt.int32
ALU = mybir.AluOpType
ACTF = mybir.ActivationFunctionType
AX = mybir.AxisListType

ALPHA = 2.0 ** 0.25
LN_EPS = 1e-5
NEXP = 64
WINDOWS = (2, 4, 8, 16)
NSTREAM = 80
RING = 8


class Buf:
    __slots__ = ("name", "w", "r", "dsem", "dcount")

    def __init__(self, name):
        self.name = name
        self.w = None
        self.r = {}
        self.dsem = {}
        self.dcount = {}


class EngState:
    def __init__(self, name, sem):
        self.name = name
        self.sem = sem
        self.count = 0
        self.seen = {}
        self.ninst = 0
        self.prog = []


class Sched:
    def __init__(self, nc, stack):
        self.nc = nc
        self.stack = stack
        self.eng = {}
        for name in ("pe", "dve", "act", "pool", "sp"):
            sem = stack.enter_context(nc.semaphore("sem_" + name))
            self.eng[name] = EngState(name, sem)
        self.bufs = []
        self.nsem = 5

    def buf(self, name):
        b = Buf(name)
        self.bufs.append(b)
        return b

    def _wait_deps(self, e, reads, writes, skip_own=False, skip_key=None):
        need = {}

        def add(k, sem, val):
            if k not in need or need[k][1] < val:
                need[k] = (sem, val)

        for b in reads:
            if b.w is not None:
                add(*b.w)
        for b in writes:
            if b.w is not None:
                add(*b.w)
            for k, (sem, val) in b.r.items():
                add(k, sem, val)
        for k, (sem, val) in need.items():
            if (skip_own and k == e.name) or k == skip_key:
                continue
            if e.seen.get(k, 0) < val:
                e.prog.append(("w", sem, val))
                e.seen[k] = val

    def _mark(self, tok, reads, writes):
        k, sem, val = tok
        for b in reads:
            if k not in b.r or b.r[k][1] < val:
                b.r[k] = (sem, val)
        for b in writes:
            b.w = tok
            b.r = {}

    def op(self, eng, fn, reads=(), writes=()):
        e = self.eng[eng]
        self._wait_deps(e, reads, writes, skip_own=(eng == "pe"))
        e.count += 1
        e.ninst += 1
        e.prog.append(("i", fn, e.sem, 1))
        self._mark((e.name, e.sem, e.count), reads, writes)

    def mm_group(self, fns, reads=(), writes=()):
        e = self.eng["pe"]
        self._wait_deps(e, reads, writes, skip_own=True)
        for fn in fns[:-1]:
            e.prog.append(("i", fn, None, 0))
            e.ninst += 1
        e.ninst += 1
        e.count += 1
        e.prog.append(("i", fns[-1], e.sem, 1))
        self._mark((e.name, e.sem, e.count), reads, writes)

    def _dsem(self, b, kind):
        if kind not in b.dsem:
            b.dsem[kind] = self.stack.enter_context(self.nc.semaphore("dsem_" + kind + "_" + b.name))
            b.dcount[kind] = 0
            self.nsem += 1
        return b.dsem[kind]

    def dma(self, queue, fn, owner, reads=(), writes=()):
        e = self.eng[queue]
        kind = "sw" if queue == "pool" else "hw"
        self._wait_deps(e, reads, writes, skip_key="d" + kind + "_" + owner.name)
        e.ninst += 1
        sem = self._dsem(owner, kind)
        owner.dcount[kind] += 16
        e.prog.append(("i", fn, sem, 16))
        self._mark(("d" + kind + "_" + owner.name, sem, owner.dcount[kind]), reads, writes)

    def barrier(self):
        for e in self.eng.values():
            for o in self.eng.values():
                if o.count > 0 and e.seen.get(o.name, 0) < o.count:
                    e.prog.append(("w", o.sem, o.count))
                    e.seen[o.name] = o.count
            for b in self.bufs:
                for kind, sem in b.dsem.items():
                    k = "d" + kind + "_" + b.name
                    if e.seen.get(k, 0) < b.dcount[kind]:
                        e.prog.append(("w", sem, b.dcount[kind]))
                        e.seen[k] = b.dcount[kind]
        for b in self.bufs:
            b.w = None
            b.r = {}

    def emit(self):
        with self.nc.Block() as block:
            def run(e):
                def body(h):
                    for it in e.prog:
                        if it[0] == "w":
                            h.wait_ge(it[1], it[2])
                        else:
                            inst = it[1](h)
                            if it[2] is not None:
                                inst.then_inc(it[2], it[3])
                return body
            block.tensor(run(self.eng["pe"]))
            block.vector(run(self.eng["dve"]))
            block.scalar(run(self.eng["act"]))
            block.gpsimd(run(self.eng["pool"]))
            block.sync(run(self.eng["sp"]))


class Arena:
    def __init__(self, ap_bf16, nelem):
        self.ap = ap_bf16
        self.n = nelem
        self.off = 0

    def alloc(self, free_shape, dtype):
        n = int(np.prod(free_shape))
        width = n * (2 if dtype in (F32, I32) else 1)
        self.off = (self.off + 15) // 16 * 16
        assert self.off + width <= self.n, f"arena overflow {self.off}+{width}>{self.n}"
        v = self.ap[:, self.off:self.off + width]
        self.off += width
        if dtype != BF16:
            v = v.bitcast(dtype)
        if len(free_shape) == 2:
            v = v.rearrange("p (a b) -> p a b", a=free_shape[0])
        elif len(free_shape) == 3:
            v = v.rearrange("p (a b c) -> p a b c", a=free_shape[0], b=free_shape[1])
        return v

    def reset(self, off=0):
        self.off = off


def build_program(S=4096, CAP=192, phases="ABC"):
    assert S % 512 == 0 and CAP in (128, 192, 256)
    NBLK = S // 512
    NTT = S // 128
    NSLOT = NEXP * CAP
    nc = bass.Bass("TRN2", target_bir_lowering=False)

    def din(name, shape, dt=F32):
        return nc.dram_tensor(name, shape, dt, kind="ExternalInput").ap()

    x_d = din("x", [S, 1024])
    mem_d = din("mem", [256, 1024])
    wst_d = din("wst", [NSTREAM, 128, 1024])
    wo_d = din("w_o", [1024, 1024])
    wkv_d = din("w_kv", [1024, 2048])
    wpool_d = din("w_pool", [4, 256, 256])
    cst_d = din("cst", [128, 64])
    aux_d = din("aux", [128, 512])
    lnp_d = din("lnp", [4, 128, 1024])
    rbias_d = din("rbias", [128, 72])
    wr_d = din("w_r", [1024, 72])
    wup_d = din("w_up", [NEXP, 1024, 1024])
    wdn_d = din("w_down", [NEXP, 512, 1024])
    out_d = nc.dram_tensor("out", [S, 1024], F32, kind="ExternalOutput").ap()
    h1_d = nc.dram_tensor("h1_scr", [S, 1024], F32).ap()
    xbuf_d = nc.dram_tensor("xbuf_scr", [NSLOT, 1024], BF16).ap()
    ybuf_d = nc.dram_tensor("ybuf_scr", [NSLOT, 1024], F32).ap()

    with ExitStack() as st:
        S_ = Sched(nc, st)
        sb = lambda name, shape, dt: st.enter_context(nc.sbuf_tensor("sb_" + name, shape, dt))
        ps = st.enter_context(nc.psum_tensor("ps", [128, 8, 512], F32))
        ps_b = [S_.buf(f"ps{i}") for i in range(8)]
        bank_ctr = [0]

        def bank():
            i = bank_ctr[0] % 8
            bank_ctr[0] += 1
            return i

        aux = sb("aux", [128, 512], F32)
        ident_f = aux[:, 0:128]
        triu_f = aux[:, 128:256]
        ones_f = aux[:, 256:384]
        eoff = aux[:, 384:448]
        invcnt = aux[:, 448:512]
        cst = sb("cst", [128, 64], F32)
        ident_b = sb("ident_b", [128, 128], BF16)
        ones_b = sb("ones_b", [128, 128], BF16)
        rbias = sb("rbias", [128, 72], F32)
        wr = sb("wr", [128, 8, 72], F32)
        desti = sb("desti", [128, 2 * NTT], I32)
        wts = sb("wts", [128, 2 * NTT], F32)
        baseoff = sb("baseoff", [128, 64], F32)
        const_b = S_.buf("const")
        identb_b = S_.buf("identb")
        desti_b = [S_.buf(f"desti{g}") for g in range(NTT)]
        wts_b = [S_.buf(f"wts{g}") for g in range(NTT)]
        baseoff_b = S_.buf("baseoff")
        ARENA_N = 100864
        arena_t = sb("arena", [128, ARENA_N], BF16)
        AR = Arena(arena_t, ARENA_N)

        cl = [(aux[:], aux_d), (cst[:], cst_d), (rbias[:], rbias_d)]
        for kh in range(8):
            cl.append((wr[:, kh, :], wr_d[kh * 128:(kh + 1) * 128, :]))
        for (o, i) in cl:
            S_.dma("sp", lambda h, o=o, i=i: h.dma_start(out=o, in_=i), const_b, writes=[const_b])
        S_.op("dve", lambda h: h.tensor_copy(out=ident_b[:], in_=ident_f), reads=[const_b], writes=[identb_b])
        S_.op("dve", lambda h: h.tensor_copy(out=ones_b[:], in_=ones_f), reads=[const_b], writes=[identb_b])
        S_.op("dve", lambda h: h.tensor_copy(out=baseoff[:], in_=eoff), reads=[const_b], writes=[baseoff_b])

        lnp = AR.alloc([2, 1024], F32)
        for i in range(2):
            S_.dma("sp", lambda h, i=i: h.dma_start(out=lnp[:, i, :], in_=lnp_d[i]), const_b, writes=[const_b])
        wo_sb = AR.alloc([8, 1024], BF16)
        wpool_sb = AR.alloc([4, 2, 256], BF16)
        kT = AR.alloc([8, 256], BF16)
        vv = AR.alloc([2, 1024], BF16)
        wo_b = S_.buf("wo"); wpool_b = S_.buf("wpool"); kT_b = S_.buf("kT"); vv_b = S_.buf("vv")
        for kh in range(8):
            S_.dma("pool", lambda h, kh=kh: h.dma_start(out=wo_sb[:, kh, :], in_=wo_d[kh * 128:(kh + 1) * 128, :]),
                   wo_b, writes=[wo_b])
        for g in range(4):
            for k2 in range(2):
                S_.dma("pool", lambda h, g=g, k2=k2: h.dma_start(out=wpool_sb[:, g, k2, :],
                                                               in_=wpool_d[g, k2 * 128:(k2 + 1) * 128, :]),
                       wpool_b, writes=[wpool_b])
        a_mark = AR.off

        wkv_sb = AR.alloc([8, 2048], BF16)
        mem_bf = AR.alloc([2, 1024], BF16)
        memT = AR.alloc([8, 256], BF16)
        wkv_b = S_.buf("wkv"); membf_b = S_.buf("membf"); memT_b = S_.buf("memT")
        for mc in range(2):
            S_.dma("pool", lambda h, mc=mc: h.dma_start(out=mem_bf[:, mc, :], in_=mem_d[mc * 128:(mc + 1) * 128, :]),
                   membf_b, writes=[membf_b])
        for kh in range(8):
            for hf in range(2):
                S_.dma("pool", lambda h, kh=kh, hf=hf: h.dma_start(
                    out=wkv_sb[:, kh, hf * 1024:(hf + 1) * 1024],
                    in_=wkv_d[kh * 128:(kh + 1) * 128, hf * 1024:(hf + 1) * 1024]), wkv_b, writes=[wkv_b])
        for mc in range(2):
            bk = bank()
            pb = ps[:, bk, :].bitcast(BF16)
            fns = [lambda h, kh=kh, mc=mc, pb=pb: h.transpose(out=pb[:, kh * 128:(kh + 1) * 128],
                                                            in_=mem_bf[:, mc, kh * 128:(kh + 1) * 128],
                                                            identity=ident_b[:]) for kh in range(8)]
            S_.mm_group(fns, reads=[membf_b, identb_b], writes=[ps_b[bk]])
            S_.op("dve", lambda h, pb=pb, mc=mc: h.tensor_copy(
                out=memT[:, :, mc * 128:(mc + 1) * 128], in_=pb.rearrange("p (k t) -> p k t", k=8)),
                reads=[ps_b[bk]], writes=[memT_b])
        for hc in range(8):
            bk = bank()
            fns = [lambda h, kh=kh, hc=hc, bk=bk: h.matmul(ps[:, bk, 0:256], lhsT=wkv_sb[:, kh, hc * 128:(hc + 1) * 128],
                                                         rhs=memT[:, kh, :], start=(kh == 0), stop=(kh == 7))
                   for kh in range(8)]
            S_.mm_group(fns, reads=[wkv_b, memT_b], writes=[ps_b[bk]])
            S_.op("act", lambda h, hc=hc, bk=bk: h.copy(out=kT[:, hc, :], in_=ps[:, bk, 0:256]),
                  reads=[ps_b[bk]], writes=[kT_b])
        for mc in range(2):
            for hf in range(2):
                bk = bank()
                fns = [lambda h, kh=kh, mc=mc, hf=hf, bk=bk: h.matmul(
                    ps[:, bk, :], lhsT=memT[:, kh, mc * 128:(mc + 1) * 128],
                    rhs=wkv_sb[:, kh, 1024 + hf * 512:1024 + (hf + 1) * 512], start=(kh == 0), stop=(kh == 7))
                    for kh in range(8)]
                S_.mm_group(fns, reads=[wkv_b, memT_b], writes=[ps_b[bk]])
                S_.op("dve", lambda h, mc=mc, hf=hf, bk=bk: h.tensor_copy(out=vv[:, mc, hf * 512:(hf + 1) * 512],
                                                                        in_=ps[:, bk, :]),
                      reads=[ps_b[bk]], writes=[vv_b])
        S_.barrier()
        AR.reset(a_mark)

        ring = AR.alloc([RING, 1024], BF16)
        ring_b = [S_.buf(f"ring{i}") for i in range(RING)]
        NXF = 2
        xf = [AR.alloc([1024], F32) for _ in range(NXF)]
        xf_b = [S_.buf(f"xf{i}") for i in range(NXF)]
        xb = [AR.alloc([1024], BF16) for _ in range(2)]
        xb_b = [S_.buf(f"xb{i}") for i in range(2)]
        xT = [AR.alloc([8, 512], BF16) for _ in range(2)]
        xT_b = [S_.buf(f"xT{i}") for i in range(2)]
        bvT = AR.alloc([8, 512], BF16); bvT_b = [S_.buf(f"bvT{i}") for i in range(8)]
        dT = AR.alloc([8, 512], BF16); dT_b = [S_.buf(f"dT{i}") for i in range(8)]
        oT = AR.alloc([8, 512], BF16); oT_b = [S_.buf(f"oT{i}") for i in range(8)]
        qm = AR.alloc([8, 512], BF16); qm_b = [S_.buf(f"qm{i}") for i in range(8)]
        expT = [AR.alloc([2, 512], BF16) for _ in range(2)]
        expT_b = [S_.buf(f"expT{i}") for i in range(2)]
        NCV = 2
        zh_sb = [AR.alloc([512], F32) for _ in range(NCV)]; zh_b = [S_.buf(f"zh{i}") for i in range(NCV)]
        u_sb = [AR.alloc([516], F32) for _ in range(NCV)]; u_b = [S_.buf(f"u{i}") for i in range(NCV)]
        t_sb = [AR.alloc([512], F32) for _ in range(NCV)]; t_b = [S_.buf(f"t{i}") for i in range(NCV)]
        uhalo = AR.alloc([8, 2], F32); uhalo_b = [S_.buf(f"uh{i}") for i in range(8)]
        p_sb = [AR.alloc([528], F32) for _ in range(2)]; p_b = [S_.buf(f"p{i}") for i in range(2)]
        s_sb = [AR.alloc([528], F32) for _ in range(2)]; s_b = [S_.buf(f"s{i}") for i in range(2)]
        phalo = AR.alloc([8, 16], F32); phalo_b = [S_.buf(f"ph{i}") for i in range(8)]
        g_sb = [AR.alloc([3, 512], F32) for _ in range(2)]; g_b = [S_.buf(f"g{i}") for i in range(2)]
        m_sb = [AR.alloc([2, 512], F32)] * 2; m_b = [S_.buf("m0")] * 2
        rden = [AR.alloc([512], F32)] * 2; rden_b = [S_.buf("rden0")] * 2
        rh_sb = [AR.alloc([1024], F32) for _ in range(4)]; rh_b = [S_.buf(f"rh{i}") for i in range(4)]
        h1bf = [AR.alloc([1024], BF16) for _ in range(4)]; h1bf_b = [S_.buf(f"h1bf{i}") for i in range(4)]
        h1T = [AR.alloc([8, 128], F32) for _ in range(2)]; h1T_b = [S_.buf(f"h1T{i}") for i in range(2)]
        NSM = 4
        junkA = AR.alloc([1024], BF16); junkA_b = S_.buf("junkA")
        sm = [AR.alloc([512], F32) for _ in range(NSM)]
        sm_b = [S_.buf(f"sm{i}") for i in range(NSM)]
        print("phase A arena", AR.off * 2 / 1024, "KiB")

        S_.op("dve", lambda h: h.memset(uhalo[:], 0.0), writes=uhalo_b)
        S_.op("dve", lambda h: h.memset(phalo[:], 0.0), writes=phalo_b)

        stream_ctr = [0]

        def stream_next(blk):
            i = stream_ctr[0]
            stream_ctr[0] += 1
            ci = i % NSTREAM
            s = i % RING
            S_.dma("pool", lambda h, ci=ci, s=s: h.dma_start(out=ring[:, s, :], in_=wst_d[ci]), ring_b[s],
                   writes=[ring_b[s]])
            return s

        class Stream:
            def __init__(self, total):
                self.total = total
                self.issued = 0
                self.consumed = 0
                self.slots = {}

            def prime(self, n):
                while self.issued < self.total and self.issued < self.consumed + n:
                    self.slots[self.issued] = stream_next(0)
                    self.issued += 1

            def take(self):
                self.prime(1)
                s = self.slots.pop(self.consumed)
                self.consumed += 1
                return s

        stream = Stream(NSTREAM * NBLK)

        def in_proj(slot, xTi, bk):
            fns = [lambda h, kh=kh: h.matmul(ps[:, bk, :], lhsT=ring[:, slot, kh * 128:(kh + 1) * 128],
                                             rhs=xT[xTi][:, kh, :], start=(kh == 0), stop=(kh == 7))
                   for kh in range(8)]
            S_.mm_group(fns, reads=[ring_b[slot], xT_b[xTi]], writes=[ps_b[bk]])

        def load_x_tiles(g):
            pass

        def load_xb(blk, tt):
            g = blk * 4 + tt
            bi = g % 2
            S_.dma("pool", lambda h, g=g, bi=bi: h.dma_start(out=xb[bi][:], in_=x_d[g * 128:(g + 1) * 128, :]),
                   xb_b[bi], writes=[xb_b[bi]])

        def transpose_x(blk, xTi, tts=(0, 1, 2, 3), load=True):
            for tt in tts:
                g = blk * 4 + tt
                bi = g % 2
                if load:
                    load_xb(blk, tt)
                bk = bank()
                pb = ps[:, bk, :].bitcast(BF16)
                fns = [lambda h, kh=kh, bi=bi, pb=pb: h.transpose(out=pb[:, kh * 128:(kh + 1) * 128],
                                                                in_=xb[bi][:, kh * 128:(kh + 1) * 128],
                                                                identity=ident_b[:]) for kh in range(8)]
                S_.mm_group(fns, reads=[xb_b[bi], identb_b], writes=[ps_b[bk]])
                S_.op("act", lambda h, pb=pb, tt=tt, xTi=xTi: h.copy(
                    out=xT[xTi][:, :, tt * 128:(tt + 1) * 128], in_=pb.rearrange("p (k t) -> p k t", k=8)),
                    reads=[ps_b[bk]], writes=[xT_b[xTi]])

        cw = lambda k, c: cst[:, 24 + k * 8 + c:24 + k * 8 + c + 1]
        bg = lambda i, j: cst[:, i * 8 + j:i * 8 + j + 1]
        psc = lambda j: cst[:, 48 + j:48 + j + 1]

        def conv_chunk(blk, c, xTi):
            i = (blk * 8 + c) % NCV
            bkb, bkc, bkh = bank(), bank(), bank()
            for bk in (bkb, bkc, bkh):
                in_proj(stream.take(), xTi, bk)
            stream.prime(RING)
            S_.op("act", lambda h: h.copy(out=zh_sb[i][:], in_=ps[:, bkh, :]), reads=[ps_b[bkh]], writes=[zh_b[i]])
            S_.op("act", lambda h: h.copy(out=u_sb[i][:, 0:2], in_=uhalo[:, c, :]), reads=[uhalo_b[c]], writes=[u_b[i]])
            S_.op("dve", lambda h: h.tensor_tensor(out=u_sb[i][:, 2:514], in0=ps[:, bkc, :], in1=zh_sb[i][:], op=ALU.mult),
                  reads=[ps_b[bkc], zh_b[i]], writes=[u_b[i]])
            S_.op("act", lambda h: h.copy(out=uhalo[:, c, :], in_=u_sb[i][:, 512:514]), reads=[u_b[i]], writes=[uhalo_b[c]])
            S_.op("act", lambda h: h.activation(out=t_sb[i][:], in_=u_sb[i][:, 0:512], func=ACTF.Copy, scale=cw(0, c)),
                  reads=[u_b[i], const_b], writes=[t_b[i]])
            S_.op("dve", lambda h: h.scalar_tensor_tensor(out=t_sb[i][:], in0=u_sb[i][:, 1:513], scalar=cw(1, c),
                                                          in1=t_sb[i][:], op0=ALU.mult, op1=ALU.add),
                  reads=[u_b[i], t_b[i], const_b], writes=[t_b[i]])
            S_.op("dve", lambda h: h.scalar_tensor_tensor(out=t_sb[i][:], in0=u_sb[i][:, 2:514], scalar=cw(2, c),
                                                          in1=t_sb[i][:], op0=ALU.mult, op1=ALU.add),
                  reads=[u_b[i], t_b[i], const_b], writes=[t_b[i]])
            S_.op("dve", lambda h: h.tensor_tensor(out=bvT[:, c, :], in0=t_sb[i][:], in1=ps[:, bkb, :], op=ALU.mult),
                  reads=[t_b[i], ps_b[bkb]], writes=[bvT_b[c]])

        def pool_chunk(blk, c, xTi):
            i = (blk * 8 + c) % 2
            gi = c // 2
            w = WINDOWS[gi]
            bk = bank()
            in_proj(stream.take(), xTi, bk)
            stream.prime(RING)
            P = p_sb[i]
            S_.op("act", lambda h: h.copy(out=P[:, 0:16], in_=phalo[:, c, :]), reads=[phalo_b[c]], writes=[p_b[i]])
            S_.op("act", lambda h: h.copy(out=P[:, 16:528], in_=ps[:, bk, :]), reads=[ps_b[bk]], writes=[p_b[i]])
            S_.op("act", lambda h: h.copy(out=phalo[:, c, :], in_=P[:, 512:528]), reads=[p_b[i]], writes=[phalo_b[c]])
            cur, cur_b, lo, step = P, p_b[i], 0, 1
            nst = {2: 1, 4: 2, 8: 3, 16: 4}[w]
            for si in range(nst):
                dst, dst_b = s_sb[si % 2], s_b[si % 2]
                nlo = lo + step
                S_.op("dve", lambda h, cur=cur, dst=dst, lo=lo, nlo=nlo, step=step: h.tensor_tensor(
                    out=dst[:, nlo:528], in0=cur[:, nlo:528], in1=cur[:, lo:528 - step], op=ALU.add),
                    reads=[cur_b], writes=[dst_b])
                cur, cur_b, lo, step = dst, dst_b, nlo, step * 2
            S_.op("dve", lambda h, cur=cur: h.scalar_tensor_tensor(out=dT[:, c, :], in0=cur[:, 16:528], scalar=1.0 / w,
                                                                 in1=P[:, 16:528], op0=ALU.mult, op1=ALU.subtract),
                  reads=[cur_b, p_b[i]], writes=[dT_b[c]])
            if blk == 0:
                tmpb = sm_b[0]
                S_.op("dve", lambda h, cur=cur: h.tensor_tensor(out=sm[0][:, 0:16], in0=cur[:, 16:32],
                                                              in1=invcnt[:, gi * 16:(gi + 1) * 16], op=ALU.mult),
                      reads=[cur_b, const_b], writes=[tmpb])
                S_.op("dve", lambda h: h.tensor_tensor(out=dT[:, c, 0:16], in0=sm[0][:, 0:16], in1=P[:, 16:32],
                                                       op=ALU.subtract),
                      reads=[tmpb, p_b[i], dT_b[c]], writes=[dT_b[c]])

        def q_chunk(blk, c, xTi):
            bk = bank()
            in_proj(stream.take(), xTi, bk)
            stream.prime(RING)
            S_.op("act", lambda h: h.copy(out=qm[:, c, :], in_=ps[:, bk, :]), reads=[ps_b[bk]], writes=[qm_b[c]])

        def att_scores(hd):
            ei = hd % 2
            for mc in range(2):
                bk = bank()
                fns = [lambda h, hc=hc, bk=bk, mc=mc: h.matmul(ps[:, bk, :], lhsT=kT[:, 2 * hd + hc, mc * 128:(mc + 1) * 128],
                                                             rhs=qm[:, 2 * hd + hc, :], start=(hc == 0), stop=(hc == 1))
                       for hc in range(2)]
                S_.mm_group(fns, reads=[kT_b, qm_b[2 * hd], qm_b[2 * hd + 1]], writes=[ps_b[bk]])
                S_.op("act", lambda h, bk=bk, mc=mc: h.activation(out=expT[ei][:, mc, :], in_=ps[:, bk, :], func=ACTF.Exp,
                                                                scale=1.0 / 16.0),
                      reads=[ps_b[bk]], writes=[expT_b[ei]])

        def att_av(hd):
            ei = hd % 2
            bkd = bank()
            fns = [lambda h, mc=mc: h.matmul(ps[:, bkd, :], lhsT=ones_b[:], rhs=expT[ei][:, mc, :],
                                             start=(mc == 0), stop=(mc == 1)) for mc in range(2)]
            S_.mm_group(fns, reads=[expT_b[ei], identb_b], writes=[ps_b[bkd]])
            S_.op("dve", lambda h: h.reciprocal(out=rden[ei][:], in_=ps[:, bkd, :]), reads=[ps_b[bkd]], writes=[rden_b[ei]])
            for hc in range(2):
                bk = bank()
                c = 2 * hd + hc
                fns = [lambda h, mc=mc, bk=bk, c=c: h.matmul(ps[:, bk, :], lhsT=vv[:, mc, c * 128:(c + 1) * 128],
                                                           rhs=expT[ei][:, mc, :], start=(mc == 0), stop=(mc == 1))
                       for mc in range(2)]
                S_.mm_group(fns, reads=[expT_b[ei], vv_b], writes=[ps_b[bk]])
                S_.op("dve", lambda h, bk=bk, c=c: h.tensor_tensor(out=oT[:, c, :], in0=ps[:, bk, :], in1=rden[ei][:],
                                                                 op=ALU.mult),
                      reads=[ps_b[bk], rden_b[ei]], writes=[oT_b[c]])

        def merge_chunk(blk, j, xTi):
            i = (blk * 8 + j) % 2
            G, M = g_sb[i], m_sb[i]
            for br in range(3):
                bk = bank()
                in_proj(stream.take(), xTi, bk)
                S_.op("act", lambda h, bk=bk, br=br: h.activation(out=G[:, br, :], in_=ps[:, bk, :], func=ACTF.Sigmoid,
                                                                bias=bg(br, j), scale=1.0),
                      reads=[ps_b[bk], const_b], writes=[g_b[i]])
            s_co = stream.take()
            bkc = bank()
            fns = [lambda h, kh=kh: h.matmul(ps[:, bkc, :], lhsT=ring[:, s_co, kh * 128:(kh + 1) * 128], rhs=bvT[:, kh, :],
                                             start=(kh == 0), stop=(kh == 7)) for kh in range(8)]
            S_.mm_group(fns, reads=[ring_b[s_co]] + bvT_b, writes=[ps_b[bkc]])
            gi = j // 2
            bkp = bank()
            fns = [lambda h, k2=k2: h.matmul(ps[:, bkp, :], lhsT=wpool_sb[:, gi, k2, (j % 2) * 128:(j % 2 + 1) * 128],
                                             rhs=dT[:, 2 * gi + k2, :], start=(k2 == 0), stop=(k2 == 1)) for k2 in range(2)]
            S_.mm_group(fns, reads=[wpool_b, dT_b[2 * gi], dT_b[2 * gi + 1]], writes=[ps_b[bkp]])
            s_xo = stream.take()
            bkm = bank()
            fns = [lambda h, kh=kh: h.matmul(ps[:, bkm, :], lhsT=ring[:, s_xo, kh * 128:(kh + 1) * 128], rhs=oT[:, kh, :],
                                             start=(kh == 0), stop=(kh == 7)) for kh in range(8)]
            S_.mm_group(fns, reads=[ring_b[s_xo]] + oT_b, writes=[ps_b[bkm]])
            stream.prime(RING)
            S_.op("dve", lambda h: h.tensor_tensor(out=M[:, 0, :], in0=G[:, 0, :], in1=ps[:, bkc, :], op=ALU.mult),
                  reads=[g_b[i], ps_b[bkc]], writes=[m_b[i]])
            S_.op("dve", lambda h: h.scalar_tensor_tensor(out=M[:, 1, :], in0=ps[:, bkp, :], scalar=psc(j), in1=G[:, 1, :],
                                                          op0=ALU.mult, op1=ALU.mult),
                  reads=[g_b[i], ps_b[bkp], const_b], writes=[m_b[i]])
            S_.op("dve", lambda h: h.tensor_tensor(out=M[:, 0, :], in0=M[:, 0, :], in1=M[:, 1, :], op=ALU.add),
                  reads=[m_b[i]], writes=[m_b[i]])
            S_.op("dve", lambda h: h.tensor_tensor(out=M[:, 1, :], in0=G[:, 2, :], in1=ps[:, bkm, :], op=ALU.mult),
                  reads=[g_b[i], ps_b[bkm], m_b[i]], writes=[m_b[i]])
            S_.op("dve", lambda h: h.tensor_tensor(out=qm[:, j, :], in0=M[:, 0, :], in1=M[:, 1, :], op=ALU.add),
                  reads=[m_b[i]], writes=[qm_b[j]])

        def ln_stats_on(R, Rb, sm_t, smb, junk, junk_b):
            S_.op("act", lambda h: h.activation(out=junk[:], in_=R[:], func=ACTF.Identity, accum_out=sm_t[:, 0:1]),
                  reads=[Rb], writes=[junk_b, smb])
            S_.op("act", lambda h: h.activation(out=junk[:], in_=R[:], func=ACTF.Square, accum_out=sm_t[:, 1:2]),
                  reads=[Rb], writes=[junk_b, smb])
            S_.op("dve", lambda h: h.tensor_scalar(out=sm_t[:, 12:13], in0=sm_t[:, 0:1], scalar1=1.0 / 1024.0, scalar2=None,
                                                   op0=ALU.mult), reads=[smb], writes=[smb])
            S_.op("dve", lambda h: h.tensor_tensor(out=sm_t[:, 2:3], in0=sm_t[:, 12:13], in1=sm_t[:, 12:13], op=ALU.mult),
                  reads=[smb], writes=[smb])
            S_.op("dve", lambda h: h.scalar_tensor_tensor(out=sm_t[:, 13:14], in0=sm_t[:, 1:2], scalar=1.0 / 1024.0,
                                                          in1=sm_t[:, 2:3], op0=ALU.mult, op1=ALU.subtract),
                  reads=[smb], writes=[smb])
            S_.op("act", lambda h: h.activation(out=sm_t[:, 14:15], in_=sm_t[:, 13:14], func=ACTF.Sqrt, bias=eps_ap,
                                                scale=1.0), reads=[smb, const_b], writes=[smb])
            S_.op("dve", lambda h: h.reciprocal(out=sm_t[:, 15:16], in_=sm_t[:, 14:15]), reads=[smb], writes=[smb])
            S_.op("dve", lambda h: h.tensor_scalar(out=sm_t[:, 16:17], in0=sm_t[:, 12:13], scalar1=-1.0,
                                                   scalar2=sm_t[:, 15:16], op0=ALU.mult, op1=ALU.mult),
                  reads=[smb], writes=[smb])
            return sm_t[:, 15:16], sm_t[:, 16:17]

        def ln_stats(R, Rb, idx):
            return ln_stats_on(R, Rb, sm[idx], sm_b[idx], junkA, junkA_b)

        eps_ap = cst[:, 56:57]

        NH = 4

        def stage1(blk, tt):
            g = blk * 4 + tt
            i = g % NH
            fi = g % NXF
            S_.dma("sp", lambda h: h.dma_start(out=xf[fi][:], in_=x_d[g * 128:(g + 1) * 128, :]), xf_b[fi], writes=[xf_b[fi]])
            R = rh_sb[i]
            for hf in range(2):
                bk = bank()
                fns = [lambda h, kh=kh, bk=bk, hf=hf: h.matmul(ps[:, bk, :], lhsT=qm[:, kh, tt * 128:(tt + 1) * 128],
                                                             rhs=wo_sb[:, kh, hf * 512:(hf + 1) * 512],
                                                             start=(kh == 0), stop=(kh == 7)) for kh in range(8)]
                S_.mm_group(fns, reads=qm_b + [wo_b], writes=[ps_b[bk]])
                S_.op("dve", lambda h, bk=bk, hf=hf: h.scalar_tensor_tensor(
                    out=R[:, hf * 512:(hf + 1) * 512], in0=xf[fi][:, hf * 512:(hf + 1) * 512], scalar=ALPHA,
                    in1=ps[:, bk, :], op0=ALU.mult, op1=ALU.add), reads=[xf_b[fi], ps_b[bk]], writes=[rh_b[i]])
            rstd, nmr = ln_stats(R, rh_b[i], i)
            S_.op("act", lambda h: h.activation(out=R[:], in_=R[:], func=ACTF.Identity, bias=nmr, scale=rstd),
                  reads=[rh_b[i], sm_b[i]], writes=[rh_b[i]])
            S_.op("dve", lambda h: h.tensor_tensor(out=R[:], in0=R[:], in1=lnp[:, 0, :], op=ALU.mult),
                  reads=[rh_b[i], const_b], writes=[rh_b[i]])
            S_.op("dve", lambda h: h.tensor_tensor(out=R[:], in0=R[:], in1=lnp[:, 1, :], op=ALU.add),
                  reads=[rh_b[i], const_b], writes=[rh_b[i]])
            S_.op("act", lambda h: h.copy(out=h1bf[i][:], in_=R[:]), reads=[rh_b[i]], writes=[h1bf_b[i]])
            S_.dma("sp", lambda h: h.dma_start(out=h1_d[g * 128:(g + 1) * 128, :], in_=R[:]), rh_b[i], reads=[rh_b[i]])

        def stage2a(g):
            i = g % NH
            ti = g % 2
            H = rh_sb[i]
            for hf in range(2):
                bk = bank()
                fns = [lambda h, k4=k4, bk=bk, hf=hf: h.transpose(out=ps[:, bk, k4 * 128:(k4 + 1) * 128],
                                                                in_=H[:, (hf * 4 + k4) * 128:(hf * 4 + k4 + 1) * 128],
                                                                identity=ident_f) for k4 in range(4)]
                S_.mm_group(fns, reads=[rh_b[i], const_b], writes=[ps_b[bk]])
                S_.op("act", lambda h, bk=bk, hf=hf: h.copy(out=h1T[ti][:, hf * 4:(hf + 1) * 4, :],
                                                          in_=ps[:, bk, :].rearrange("p (k t) -> p k t", k=4)),
                      reads=[ps_b[bk]], writes=[h1T_b[ti]])

        def stage2b(g):
            i = g % NH
            ti = g % 2
            bk = bank()
            fns = [lambda h, kh=kh: h.matmul(ps[:, bk, 0:72], lhsT=h1T[ti][:, kh, :], rhs=wr[:, kh, :],
                                             start=(kh == 0), stop=(kh == 7)) for kh in range(8)]
            S_.mm_group(fns, reads=[h1T_b[ti], const_b], writes=[ps_b[bk]])
            S_.op("dve", lambda h: h.tensor_tensor(out=sm[i][:, 20:92], in0=ps[:, bk, 0:72], in1=rbias[:], op=ALU.add),
                  reads=[ps_b[bk], const_b, sm_b[i]], writes=[sm_b[i]])

        def stage3a(g):
            route(g, g % NH, "a")

        def stage3b(g):
            route(g, g % NH, "b")

        def route(g, i, part):
            T, Tb = sm[i], sm_b[i]
            Lb = T[:, 20:92]; gm = T[:, 92:100]; pen = T[:, 100:108]; em = T[:, 108:172]; oh1 = T[:, 172:236]
            em2 = T[:, 236:300]; oh2 = T[:, 300:364]; A = T[:, 364:428]; pos = T[:, 428:492]
            sc = lambda k: T[:, 492 + k:493 + k]
            gmax, ngmax, gsum, gw, m1, m2, dl, e2, den, rd, d1, d2 = [sc(k) for k in range(12)]
            gexp = T[:, 504:512]
            o = lambda fn, reads=(), writes=(): S_.op("dve", fn, reads=[Tb] + list(reads), writes=[Tb] + list(writes))
            if part == "a":
                route_a(o, Tb, Lb, gm, pen, em, oh1, em2, oh2, A, gmax, ngmax, gsum, gw, m1, m2, gexp)
            else:
                route_b(o, g, i, Tb, oh1, oh2, A, pos, gw, m1, m2, dl, e2, den, rd, d1, d2)

        def route_a(o, Tb, Lb, gm, pen, em, oh1, em2, oh2, A, gmax, ngmax, gsum, gw, m1, m2, gexp):
            o(lambda h: h.reduce_max(out=gmax, in_=Lb[:, 0:8], axis=AX.X))
            o(lambda h: h.tensor_scalar(out=ngmax, in0=gmax, scalar1=-1.0, scalar2=None, op0=ALU.mult))
            S_.op("act", lambda h: h.activation(out=gexp, in_=Lb[:, 0:8], func=ACTF.Exp, bias=ngmax, scale=1.0,
                                                accum_out=gsum), reads=[Tb], writes=[Tb])
            o(lambda h: h.reciprocal(out=gw, in_=gsum))
            o(lambda h: h.tensor_scalar(out=gm, in0=Lb[:, 0:8], scalar1=gmax, scalar2=None, op0=ALU.is_equal))
            o(lambda h: h.tensor_scalar(out=pen, in0=gm, scalar1=-1.0, scalar2=1.0e30, op0=ALU.add, op1=ALU.mult))
            o(lambda h: h.tensor_tensor(out=em.rearrange("p (g j) -> p g j", g=8),
                                        in0=Lb[:, 8:72].rearrange("p (g j) -> p g j", g=8),
                                        in1=pen.unsqueeze(2).to_broadcast([128, 8, 8]), op=ALU.add))
            o(lambda h: h.reduce_max(out=m1, in_=em, axis=AX.X))
            o(lambda h: h.tensor_scalar(out=oh1, in0=em, scalar1=m1, scalar2=None, op0=ALU.is_equal))
            o(lambda h: h.scalar_tensor_tensor(out=em2, in0=oh1, scalar=-1.0e30, in1=em, op0=ALU.mult, op1=ALU.add))
            o(lambda h: h.reduce_max(out=m2, in_=em2, axis=AX.X))
            o(lambda h: h.tensor_scalar(out=oh2, in0=em2, scalar1=m2, scalar2=None, op0=ALU.is_equal))
            o(lambda h: h.tensor_tensor(out=A, in0=oh1, in1=oh2, op=ALU.add))

        def route_b(o, g, i, Tb, oh1, oh2, A, pos, gw, m1, m2, dl, e2, den, rd, d1, d2):
            T = sm[i]
            bk = bank()
            S_.mm_group([lambda h: h.matmul(ps[:, bk, 0:64], lhsT=triu_f, rhs=A, start=True, stop=True),
                         lambda h: h.matmul(ps[:, bk, 64:128], lhsT=ones_f, rhs=A, start=True, stop=True)],
                        reads=[Tb, const_b], writes=[ps_b[bk]])
            o(lambda h: h.tensor_tensor(out=pos, in0=ps[:, bk, 0:64], in1=baseoff[:], op=ALU.add),
              reads=[ps_b[bk], baseoff_b])
            S_.op("dve", lambda h: h.tensor_tensor(out=baseoff[:], in0=ps[:, bk, 64:128], in1=baseoff[:], op=ALU.add),
                  reads=[ps_b[bk], baseoff_b], writes=[baseoff_b])
            o(lambda h: h.tensor_tensor(out=oh1, in0=oh1, in1=pos, op=ALU.mult))
            o(lambda h: h.reduce_sum(out=d1, in_=oh1, axis=AX.X))
            o(lambda h: h.tensor_tensor(out=oh2, in0=oh2, in1=pos, op=ALU.mult))
            o(lambda h: h.reduce_sum(out=d2, in_=oh2, axis=AX.X))
            o(lambda h: h.tensor_copy(out=desti[:, 2 * g:2 * g + 1], in_=d1), writes=[desti_b[g]])
            o(lambda h: h.tensor_copy(out=desti[:, 2 * g + 1:2 * g + 2], in_=d2), writes=[desti_b[g]])
            o(lambda h: h.tensor_tensor(out=dl, in0=m2, in1=m1, op=ALU.subtract))
            S_.op("act", lambda h: h.activation(out=e2, in_=dl, func=ACTF.Exp), reads=[Tb], writes=[Tb])
            o(lambda h: h.tensor_scalar(out=den, in0=e2, scalar1=1.0, scalar2=None, op0=ALU.add))
            o(lambda h: h.reciprocal(out=rd, in_=den))
            o(lambda h: h.tensor_tensor(out=wts[:, 2 * g:2 * g + 1], in0=rd, in1=gw, op=ALU.mult), writes=[wts_b[g]])
            o(lambda h: h.tensor_tensor(out=wts[:, 2 * g + 1:2 * g + 2], in0=wts[:, 2 * g:2 * g + 1], in1=e2, op=ALU.mult),
              reads=[wts_b[g]], writes=[wts_b[g]])
            for k in range(2):
                S_.dma("pool", lambda h, k=k: h.indirect_dma_start(
                    out=xbuf_d, out_offset=bass.IndirectOffsetOnAxis(ap=desti[:, 2 * g + k:2 * g + k + 1], axis=0),
                    in_=h1bf[i][:], in_offset=None), h1bf_b[i], reads=[h1bf_b[i], desti_b[g]])

        xbuf_b = S_.buf("xbuf_dram")
        ybuf_b = S_.buf("ybuf_dram")

        stream.prime(RING)
        transpose_x(0, 0)
        pending = []

        def flush(n):
            for _ in range(min(n, len(pending))):
                for f in pending.pop(0):
                    f()

        def tail_schedule(blk):
            g0 = blk * 4
            S1 = lambda t: (lambda: stage1(blk, t))
            A2 = lambda t: (lambda: stage2a(g0 + t))
            B2 = lambda t: (lambda: stage2b(g0 + t))
            A3 = lambda t: (lambda: stage3a(g0 + t))
            B3 = lambda t: (lambda: stage3b(g0 + t))
            return [[S1(0)], [S1(1)], [S1(2), A2(0)], [S1(3), B2(0), A2(1)], [A3(0), B2(1), A2(2)],
                    [B2(2), A3(1), A2(3)], [B3(0), B2(3), A3(2)], [B3(1), A3(3)], [B3(2)], [B3(3)]]

        for blk in range(NBLK):
            xTi = blk % 2
            for c in range(8):
                conv_chunk(blk, c, xTi)
                flush(1)
            for c in range(8):
                pool_chunk(blk, c, xTi)
                flush(1)
            flush(len(pending))
            for c in range(8):
                q_chunk(blk, c, xTi)
            att_scores(0)
            att_scores(1)
            att_av(0)
            att_scores(2)
            att_av(1)
            att_scores(3)
            att_av(2)
            att_av(3)
            if blk + 1 < NBLK:
                load_xb(blk + 1, 0)
                load_xb(blk + 1, 1)
            for j in range(8):
                merge_chunk(blk, j, xTi)
                if blk + 1 < NBLK and j == 2:
                    transpose_x(blk + 1, (blk + 1) % 2, tts=(0, 1), load=False)
                    load_xb(blk + 1, 2)
                    load_xb(blk + 1, 3)
                if blk + 1 < NBLK and j == 5:
                    transpose_x(blk + 1, (blk + 1) % 2, tts=(2, 3), load=False)
            pending += tail_schedule(blk)
        flush(len(pending))
        S_.barrier()
        if "B" not in phases:
            for g in range(NTT):
                i = g % 2
                S_.dma("sp", lambda h, g=g, i=i: h.dma_start(out=rh_sb[i][:], in_=h1_d[g * 128:(g + 1) * 128, :]),
                       rh_b[i], writes=[rh_b[i]])
                S_.dma("sp", lambda h, g=g, i=i: h.dma_start(out=out_d[g * 128:(g + 1) * 128, :], in_=rh_sb[i][:]),
                       rh_b[i], reads=[rh_b[i]])
            S_.barrier()
            S_.emit()
            print("insts", {k: e.ninst for k, e in S_.eng.items()}, "nsem", S_.nsem)
            return nc

        AR.reset(0)
        NW = 3
        wup = [AR.alloc([8, 1024], BF16) for _ in range(NW)]; wup_b = [S_.buf(f"wup{i}") for i in range(NW)]
        wdn = [AR.alloc([4, 1024], BF16) for _ in range(NW)]; wdn_b = [S_.buf(f"wdn{i}") for i in range(NW)]
        NT2 = (CAP + 127) // 128
        rows_of = [min(128, CAP - t * 128) for t in range(NT2)]
        xg = [[AR.alloc([1024], BF16) for _ in range(NT2)] for _ in range(2)]
        xg_b = [[S_.buf(f"xg{i}_{t}") for t in range(NT2)] for i in range(2)]
        xgT = [AR.alloc([8, CAP], BF16) for _ in range(2)]; xgT_b = [S_.buf(f"xgT{i}") for i in range(2)]
        sg = [AR.alloc([CAP], F32) for _ in range(2)]; sg_b = [S_.buf(f"sg{i}") for i in range(2)]
        actT = [AR.alloc([4, CAP], BF16) for _ in range(2)]; actT_b = [S_.buf(f"actT{i}") for i in range(2)]
        ysb = [AR.alloc([1024], F32) for _ in range(3)]; ysb_b = [S_.buf(f"ysb{i}") for i in range(3)]
        print("phase B arena", AR.off * 2 / 1024, "KiB")
        ys_ctr = [0]

        def load_expert(e):
            wi = e % NW
            for kh in range(8):
                S_.dma("pool", lambda h, kh=kh: h.dma_start(out=wup[wi][:, kh, :], in_=wup_d[e, kh * 128:(kh + 1) * 128, :]),
                       wup_b[wi], writes=[wup_b[wi]])
            for kh in range(4):
                S_.dma("pool", lambda h, kh=kh: h.dma_start(out=wdn[wi][:, kh, :], in_=wdn_d[e, kh * 128:(kh + 1) * 128, :]),
                       wdn_b[wi], writes=[wdn_b[wi]])

        def expert(e):
            wi = e % NW
            i = e % 2
            for t in range(NT2):
                rows = rows_of[t]
                S_.dma("sp", lambda h, t=t, rows=rows: h.dma_start(
                    out=xg[i][t][0:rows, :], in_=xbuf_d[e * CAP + t * 128:e * CAP + t * 128 + rows, :]),
                    xg_b[i][t], writes=[xg_b[i][t]])
                bk = bank()
                pb = ps[:, bk, :].bitcast(BF16)
                fns = [lambda h, kh=kh, t=t, rows=rows, pb=pb: h.transpose(
                    out=pb[:, kh * 128:kh * 128 + rows], in_=xg[i][t][0:rows, kh * 128:(kh + 1) * 128],
                    identity=ident_b[0:rows, 0:rows]) for kh in range(8)]
                S_.mm_group(fns, reads=[xg_b[i][t], identb_b], writes=[ps_b[bk]])
                S_.op("act" if t == 0 else "dve", lambda h, t=t, rows=rows, pb=pb: (h.copy if t == 0 else h.tensor_copy)(
                    out=xgT[i][:, :, t * 128:t * 128 + rows],
                    in_=pb.rearrange("p (k t) -> p k t", k=8)[:, :, 0:rows]),
                    reads=[ps_b[bk]], writes=[xgT_b[i]])
            for fc in range(4):
                bkg, bkv = bank(), bank()
                for (bk, f) in ((bkg, fc), (bkv, fc + 4)):
                    fns = [lambda h, kh=kh, bk=bk, f=f: h.matmul(ps[:, bk, 0:CAP], lhsT=wup[wi][:, kh, f * 128:(f + 1) * 128],
                                                               rhs=xgT[i][:, kh, :], start=(kh == 0), stop=(kh == 7))
                           for kh in range(8)]
                    S_.mm_group(fns, reads=[wup_b[wi], xgT_b[i]], writes=[ps_b[bk]])
                si = (e * 4 + fc) % 2
                S_.op("act", lambda h, si=si, bkg=bkg: h.activation(out=sg[si][:], in_=ps[:, bkg, 0:CAP], func=ACTF.Silu),
                      reads=[ps_b[bkg]], writes=[sg_b[si]])
                S_.op("dve", lambda h, si=si, fc=fc, bkv=bkv: h.tensor_tensor(out=actT[i][:, fc, :], in0=sg[si][:], in1=ps[:, bkv, 0:CAP],
                                                                   op=ALU.mult),
                      reads=[sg_b[si], ps_b[bkv]], writes=[actT_b[i]])
            for t in range(NT2):
                rows = rows_of[t]
                yi = ys_ctr[0] % 3
                ys_ctr[0] += 1
                for hf in range(2):
                    bk = bank()
                    fns = [lambda h, fc=fc, bk=bk, hf=hf, t=t, rows=rows: h.matmul(
                        ps[0:rows, bk, :], lhsT=actT[i][:, fc, t * 128:t * 128 + rows],
                        rhs=wdn[wi][:, fc, hf * 512:(hf + 1) * 512], start=(fc == 0), stop=(fc == 3)) for fc in range(4)]
                    S_.mm_group(fns, reads=[actT_b[i], wdn_b[wi]], writes=[ps_b[bk]])
                    eng = "act" if hf == 0 else "dve"
                    S_.op(eng, lambda h, bk=bk, hf=hf, rows=rows, yi=yi, eng=eng: (h.copy if eng == "act" else h.tensor_copy)(
                        out=ysb[yi][0:rows, hf * 512:(hf + 1) * 512], in_=ps[0:rows, bk, :]),
                        reads=[ps_b[bk]], writes=[ysb_b[yi]])
                S_.dma("sp", lambda h, t=t, rows=rows, yi=yi: h.dma_start(
                    out=ybuf_d[e * CAP + t * 128:e * CAP + t * 128 + rows, :], in_=ysb[yi][0:rows, :]),
                    ysb_b[yi], reads=[ysb_b[yi]])

        for e in range(min(NW - 1, NEXP)):
            load_expert(e)
        for e in range(NEXP):
            if e + NW - 1 < NEXP:
                load_expert(e + NW - 1)
            expert(e)
        S_.barrier()

        AR.reset(0)
        NC_ = 3
        y0 = [AR.alloc([1024], F32) for _ in range(NC_)]; y0_b = [S_.buf(f"y0_{i}") for i in range(NC_)]
        y1 = [AR.alloc([1024], F32) for _ in range(NC_)]; y1_b = [S_.buf(f"y1_{i}") for i in range(NC_)]
        hh = [AR.alloc([1024], F32) for _ in range(NC_)]; hh_b = [S_.buf(f"hh{i}") for i in range(NC_)]
        sm2 = [AR.alloc([32], F32) for _ in range(NC_)]; sm2_b = [S_.buf(f"sm2_{i}") for i in range(NC_)]
        lnp2 = AR.alloc([2, 1024], F32)
        lnp2_b = S_.buf("lnp2")
        for i in range(2):
            S_.dma("sp", lambda h, i=i: h.dma_start(out=lnp2[:, i, :], in_=lnp_d[2 + i]), lnp2_b, writes=[lnp2_b])
        junkC = AR.alloc([1024], BF16); junkC_b = S_.buf("junkC")
        LOOK = NC_ - 2

        def pc_load(g):
            i = g % NC_
            S_.dma("pool", lambda h: h.indirect_dma_start(
                out=y0[i][:], out_offset=None, in_=ybuf_d,
                in_offset=bass.IndirectOffsetOnAxis(ap=desti[:, 2 * g:2 * g + 1], axis=0)),
                y0_b[i], reads=[desti_b[g]], writes=[y0_b[i]])
            S_.dma("pool", lambda h: h.indirect_dma_start(
                out=y1[i][:], out_offset=None, in_=ybuf_d,
                in_offset=bass.IndirectOffsetOnAxis(ap=desti[:, 2 * g + 1:2 * g + 2], axis=0)),
                y1_b[i], reads=[desti_b[g]], writes=[y1_b[i]])
            S_.dma("sp", lambda h: h.dma_start(out=hh[i][:], in_=h1_d[g * 128:(g + 1) * 128, :]),
                   hh_b[i], writes=[hh_b[i]])

        pc_state = {}

        def pc_c1(g):
            i = g % NC_
            Y0, Y1, H = y0[i], y1[i], hh[i]
            S_.op("act", lambda h: h.activation(out=Y0[:], in_=Y0[:], func=ACTF.Copy, scale=wts[:, 2 * g:2 * g + 1]),
                  reads=[y0_b[i], wts_b[g]], writes=[y0_b[i]])
            S_.op("dve", lambda h: h.scalar_tensor_tensor(
                out=Y1[:], in0=Y1[:], scalar=wts[:, 2 * g + 1:2 * g + 2], in1=Y0[:], op0=ALU.mult, op1=ALU.add),
                reads=[y0_b[i], y1_b[i], wts_b[g]], writes=[y1_b[i]])
            S_.op("dve", lambda h: h.scalar_tensor_tensor(out=H[:], in0=H[:], scalar=ALPHA, in1=Y1[:],
                                                          op0=ALU.mult, op1=ALU.add),
                  reads=[hh_b[i], y1_b[i]], writes=[hh_b[i]])

        def pc_c2(g):
            i = g % NC_
            H, T, Tb = hh[i], sm2[i], sm2_b[i]
            for hf in range(2):
                S_.op("dve", lambda h, hf=hf: h.bn_stats(out=T[:, hf * 6:(hf + 1) * 6], in_=H[:, hf * 512:(hf + 1) * 512]),
                      reads=[hh_b[i]], writes=[Tb])
            S_.op("dve", lambda h: h.bn_aggr(out=T[:, 12:14], in_=T[:, 0:12]), reads=[Tb], writes=[Tb])
            S_.op("act", lambda h: h.activation(out=T[:, 14:15], in_=T[:, 13:14], func=ACTF.Sqrt, bias=eps_ap, scale=1.0),
                  reads=[Tb, const_b], writes=[Tb])
            S_.op("dve", lambda h: h.reciprocal(out=T[:, 15:16], in_=T[:, 14:15]), reads=[Tb], writes=[Tb])
            S_.op("dve", lambda h: h.tensor_scalar(out=T[:, 16:17], in0=T[:, 12:13], scalar1=-1.0, scalar2=T[:, 15:16],
                                                   op0=ALU.mult, op1=ALU.mult), reads=[Tb], writes=[Tb])
            pc_state[g] = (T[:, 15:16], T[:, 16:17])

        def pc_c3(g):
            i = g % NC_
            Y0, H, Tb = y0[i], hh[i], sm2_b[i]
            rstd, nmr = pc_state.pop(g)
            S_.op("act", lambda h: h.activation(out=H[:], in_=H[:], func=ACTF.Identity, bias=nmr, scale=rstd),
                  reads=[hh_b[i], Tb], writes=[hh_b[i]])
            S_.op("dve", lambda h: h.tensor_tensor(out=H[:], in0=H[:], in1=lnp2[:, 0, :], op=ALU.mult),
                  reads=[hh_b[i], lnp2_b], writes=[hh_b[i]])
            S_.op("dve", lambda h: h.tensor_tensor(out=Y0[:], in0=H[:], in1=lnp2[:, 1, :], op=ALU.add),
                  reads=[hh_b[i], lnp2_b], writes=[y0_b[i]])
            S_.dma("sp", lambda h: h.dma_start(out=out_d[g * 128:(g + 1) * 128, :], in_=Y0[:]),
                   y0_b[i], reads=[y0_b[i]])

        for g in range(NTT + 1):
            if g < NTT:
                pc_load(g)
            if g >= 1:
                pc_c1(g - 1)
                pc_c2(g - 1)
                pc_c3(g - 1)
        S_.barrier()
        S_.emit()
        print("insts", {k: e.ninst for k, e in S_.eng.items()}, "nsem", S_.nsem)
    return nc


def _chunk(w, col0):
    blk = w[:, col0:col0 + 128].reshape(8, 128, 128)
    return np.ascontiguousarray(blk.transpose(1, 0, 2).reshape(128, 1024))


def stream_order():
    order = []
    for c in range(8):
        order += [("in", c), ("in", 8 + c), ("in", 16 + c)]
    for c in range(8):
        order.append(("in", 24 + c))
    for c in range(8):
        order.append(("in", 32 + c))
    for j in range(8):
        order += [("in", 40 + j), ("in", 48 + j), ("in", 56 + j), ("co", j), ("xo", j)]
    return order


def prep_shared(inputs, CAP):
    l = 0
    w_in = np.asarray(inputs["w_in"][l], np.float32)
    wco = np.asarray(inputs["w_conv_out"][l], np.float32)
    wxo = np.asarray(inputs["w_xo"][l], np.float32)
    src = {"in": w_in, "co": wco, "xo": wxo}
    wst = np.stack([_chunk(src[k], 128 * c) for (k, c) in stream_order()], 0)
    cst = np.zeros((128, 64), np.float32)
    cst[:, 0:24] = np.asarray(inputs["b_gate"][l], np.float32).reshape(24, 128).T
    cst[:, 24:48] = np.asarray(inputs["conv_w"][l], np.float32).reshape(24, 128).T
    cst[:, 48:56] = np.asarray(inputs["pool_scale"][l], np.float32).reshape(8, 128).T
    cst[:, 56] = LN_EPS
    aux = np.zeros((128, 512), np.float32)
    aux[:, 0:128] = np.eye(128, dtype=np.float32)
    aux[:, 128:256] = np.triu(np.ones((128, 128), np.float32))
    aux[:, 256:384] = 1.0
    aux[:, 384:448] = (np.arange(64, dtype=np.float32) * CAP - 1.0)[None, :]
    for gi, w in enumerate(WINDOWS):
        aux[:, 448 + gi * 16:448 + (gi + 1) * 16] = (1.0 / np.minimum(np.arange(1, 17), w)).astype(np.float32)[None, :]
    lnp = np.stack([np.broadcast_to(np.asarray(inputs[k][l], np.float32)[None, :], (128, 1024))
                    for k in ("ln1_g", "ln1_b", "ln2_g", "ln2_b")], 0)
    rb = np.concatenate([np.asarray(inputs["b_router_group"][l], np.float32),
                         np.asarray(inputs["b_router_expert"][l], np.float32)])
    rbias = np.ascontiguousarray(np.broadcast_to(rb[None, :], (128, 72)))
    w_r = np.ascontiguousarray(np.concatenate([np.asarray(inputs["w_router_group"][l], np.float32),
                                               np.asarray(inputs["w_router_expert"][l], np.float32)], 1))
    return {
        "wst": wst, "w_o": np.ascontiguousarray(inputs["w_o"][l], dtype=np.float32),
        "w_kv": np.ascontiguousarray(inputs["w_kv"][l], dtype=np.float32),
        "w_pool": np.ascontiguousarray(inputs["w_pool"][l], dtype=np.float32),
        "cst": cst, "aux": aux, "lnp": np.ascontiguousarray(lnp), "rbias": rbias, "w_r": w_r,
        "w_up": np.ascontiguousarray(inputs["w_up"][l], dtype=np.float32),
        "w_down": np.ascontiguousarray(inputs["w_down"][l], dtype=np.float32),
    }


CAP_DEFAULT = 192
_PROG_CACHE = {}


def kernel(**inputs):
    x = np.asarray(inputs["x"], np.float32)
    mem = np.asarray(inputs["mem"], np.float32)
    B, S, D = x.shape
    CAP = CAP_DEFAULT
    shared = prep_shared(inputs, CAP)
    key = (S, CAP)
    if key not in _PROG_CACHE:
        _PROG_CACHE[key] = build_program(S, CAP)
    nc = _PROG_CACHE[key]
    in_maps = []
    for b in range(B):
        m = dict(shared)
        m["x"] = np.ascontiguousarray(x[b])
        m["mem"] = np.ascontiguousarray(mem[b])
        in_maps.append(m)
    res = run_bass_kernel_spmd(nc, in_maps, core_ids=list(range(B)))
    out = np.stack([np.asarray(res.results[b]["out"], np.float32) for b in range(B)], 0)
    return out
```
